# Optimizing a Trainium2 kernel written in Bass

```python
import math
import jax, jax.numpy as jnp
from jax import lax
import numpy as np

D_MODEL = 4096
BATCH = 4
SEQ = 2048
DEPTH = 2

N_HEADS = 32
HEAD_DIM = D_MODEL // N_HEADS
MOBA_BLOCK = 256
MOBA_TOP_K = 3
Q_CHUNK = 128
CONV_WIDTH = 3
D_FF = 7 * D_MODEL // 2
N_EXPERTS = 8
EXPERT_TOP_K = 2
D_FF_EXPERT = 11 * D_MODEL // 8
RMS_EPS = 1e-6
NEG = -1e30

kernel_name = "hybrid_moba_shortconv_moe"


def rms_norm(x, g):
    xf = x.astype(jnp.float32)
    y = xf * lax.rsqrt(jnp.mean(xf * xf, axis=-1, keepdims=True) + RMS_EPS)
    return (y * g.astype(jnp.float32)).astype(x.dtype)


def alibi_slopes(n_heads):
    return jnp.asarray(np.power(2.0, -8.0 * np.arange(1, n_heads + 1) / n_heads).astype(np.float32))


def moba_attention(q, k, v):
    b, s, h, hd = q.shape
    nb = -(-s // MOBA_BLOCK)
    sp = nb * MOBA_BLOCK
    nc = -(-s // Q_CHUNK)
    sq = nc * Q_CHUNK
    tk = min(MOBA_TOP_K, nb - 1)
    scale = 1.0 / math.sqrt(hd)
    qh = jnp.pad(q, ((0, 0), (0, sq - s), (0, 0), (0, 0))).transpose(2, 0, 1, 3)
    kh = jnp.pad(k, ((0, 0), (0, sp - s), (0, 0), (0, 0))).transpose(2, 0, 1, 3)
    vh = jnp.pad(v, ((0, 0), (0, sp - s), (0, 0), (0, 0))).transpose(2, 0, 1, 3)
    slopes = alibi_slopes(h)

    def per_head(args):
        q_h, k_h, v_h, m_h = args
        kb = k_h.reshape(b, nb, MOBA_BLOCK, hd)
        vb = v_h.reshape(b, nb, MOBA_BLOCK, hd)
        k_mean = jnp.mean(kb.astype(jnp.float32), axis=2)

        def per_chunk(c):
            start = c * Q_CHUNK
            blk = start // MOBA_BLOCK
            qc = lax.dynamic_slice_in_dim(q_h, start, Q_CHUNK, axis=1)
            t = start + jnp.arange(Q_CHUNK)
            k_own = lax.dynamic_slice_in_dim(k_h, blk * MOBA_BLOCK, MOBA_BLOCK, axis=1)
            v_own = lax.dynamic_slice_in_dim(v_h, blk * MOBA_BLOCK, MOBA_BLOCK, axis=1)
            dist_own = t[:, None] - (blk * MOBA_BLOCK + jnp.arange(MOBA_BLOCK))[None, :]
            logit_own = (jnp.einsum('bqd,bkd->bqk', qc, k_own, preferred_element_type=jnp.float32) * scale
                         - m_h * dist_own.astype(jnp.float32))
            logit_own = jnp.where(dist_own >= 0, logit_own, NEG)
            if tk == 0:
                p = jax.nn.softmax(logit_own, axis=-1).astype(v_h.dtype)
                return jnp.einsum('bqk,bkd->bqd', p, v_own)
            gate = jnp.einsum('bqd,bnd->bqn', qc.astype(jnp.float32), k_mean)
            gate = jnp.where(jnp.arange(nb) < blk, gate, -jnp.inf)
            _, idx = lax.top_k(gate, tk)
            valid = idx < blk
            k_sel = jax.vmap(lambda kk, ii: kk[ii])(kb, idx)
            v_sel = jax.vmap(lambda vv, ii: vv[ii])(vb, idx)
            pos_sel = idx[..., None] * MOBA_BLOCK + jnp.arange(MOBA_BLOCK)
            dist_sel = t[None, :, None, None] - pos_sel
            logit_sel = (jnp.einsum('bqd,bqjkd->bqjk', qc, k_sel, preferred_element_type=jnp.float32) * scale
                         - m_h * dist_sel.astype(jnp.float32))
            logit_sel = jnp.where(valid[..., None], logit_sel, NEG)
            logits = jnp.concatenate([logit_own, logit_sel.reshape(b, Q_CHUNK, tk * MOBA_BLOCK)], axis=-1)
            p = jax.nn.softmax(logits, axis=-1).astype(v_h.dtype)
            p_own = p[..., :MOBA_BLOCK]
            p_sel = p[..., MOBA_BLOCK:].reshape(b, Q_CHUNK, tk, MOBA_BLOCK)
            return (jnp.einsum('bqk,bkd->bqd', p_own, v_own)
                    + jnp.einsum('bqjk,bqjkd->bqd', p_sel, v_sel))

        outs = lax.map(per_chunk, jnp.arange(nc))
        return outs.transpose(1, 0, 2, 3).reshape(b, sq, hd)

    o = lax.map(per_head, (qh, kh, vh, slopes))
    return o.transpose(1, 2, 0, 3)[:, :s]


def moba_mixer(x, w_qkv, q_gain, k_gain, w_out):
    b, s, d = x.shape
    q, k, v = jnp.split(x @ w_qkv, 3, axis=-1)
    q = rms_norm(q.reshape(b, s, N_HEADS, HEAD_DIM), q_gain)
    k = rms_norm(k.reshape(b, s, N_HEADS, HEAD_DIM), k_gain)
    v = v.reshape(b, s, N_HEADS, HEAD_DIM)
    o = moba_attention(q, k, v)
    return o.reshape(b, s, d) @ w_out


def short_conv_mixer(x, w_in, conv_w, w_out):
    bg, cg, hx = jnp.split(x @ w_in, 3, axis=-1)
    u = cg * hx
    y = lax.conv_general_dilated(u, conv_w[:, None, :], window_strides=(1,),
                                 padding=[(CONV_WIDTH - 1, 0)],
                                 dimension_numbers=('NWC', 'WIO', 'NWC'),
                                 feature_group_count=u.shape[-1])
    return (bg * y) @ w_out


def swiglu(x, w_gate_up, w_down):
    g, u = jnp.split(x @ w_gate_up, 2, axis=-1)
    return (jax.nn.silu(g) * u) @ w_down


def moe_swiglu(x, w_router, w_gate_up, w_down):
    b, s, d = x.shape
    xt = x.reshape(b * s, d)
    logits = (xt @ w_router).astype(jnp.float32)
    top_val, top_idx = lax.top_k(logits, EXPERT_TOP_K)
    top_w = jax.nn.softmax(top_val, axis=-1)
    combine = jnp.sum(jax.nn.one_hot(top_idx, N_EXPERTS, dtype=jnp.float32) * top_w[..., None], axis=1)
    combine = combine.astype(x.dtype)
    out = combine[:, 0:1] * swiglu(xt, w_gate_up[0], w_down[0])
    for e in range(1, N_EXPERTS):
        out = out + combine[:, e:e + 1] * swiglu(xt, w_gate_up[e], w_down[e])
    return out.reshape(b, s, d)


def setup_inputs(seed: int = 0) -> dict:
    key = jax.random.key(seed)
    ks = jax.random.split(key, 20)
    n_even = (DEPTH + 1) // 2
    n_odd = DEPTH // 2
    f32 = jnp.float32

    def w(k, shape, fan_in):
        return jax.random.normal(k, shape, f32) * (fan_in ** -0.5)

    def gain(k, shape):
        return 1.0 + 0.02 * jax.random.normal(k, shape, f32)

    return {
        "x": jax.random.normal(ks[0], (BATCH, SEQ, D_MODEL), f32),
        "attn_norm": gain(ks[1], (n_even, D_MODEL)),
        "w_qkv": w(ks[2], (n_even, D_MODEL, 3 * D_MODEL), D_MODEL),
        "q_norm": gain(ks[3], (n_even, HEAD_DIM)),
        "k_norm": gain(ks[4], (n_even, HEAD_DIM)),
        "w_attn_out": w(ks[5], (n_even, D_MODEL, D_MODEL), D_MODEL),
        "ffn_norm": gain(ks[6], (n_even, D_MODEL)),
        "w_ffn_gate_up": w(ks[7], (n_even, D_MODEL, 2 * D_FF), D_MODEL),
        "w_ffn_down": w(ks[8], (n_even, D_FF, D_MODEL), D_FF),
        "conv_norm": gain(ks[9], (n_odd, D_MODEL)),
        "w_conv_in": w(ks[10], (n_odd, D_MODEL, 3 * D_MODEL), D_MODEL),
        "conv_w": w(ks[11], (n_odd, CONV_WIDTH, D_MODEL), CONV_WIDTH),
        "w_conv_out": w(ks[12], (n_odd, D_MODEL, D_MODEL), D_MODEL),
        "moe_norm": gain(ks[13], (n_odd, D_MODEL)),
        "w_router": w(ks[14], (n_odd, D_MODEL, N_EXPERTS), D_MODEL),
        "w_expert_gate_up": w(ks[15], (n_odd, N_EXPERTS, D_MODEL, 2 * D_FF_EXPERT), D_MODEL),
        "w_expert_down": w(ks[16], (n_odd, N_EXPERTS, D_FF_EXPERT, D_MODEL), D_FF_EXPERT),
    }


def reference(x, attn_norm, w_qkv, q_norm, k_norm, w_attn_out, ffn_norm, w_ffn_gate_up, w_ffn_down,
              conv_norm, w_conv_in, conv_w, w_conv_out, moe_norm, w_router, w_expert_gate_up, w_expert_down):
    h = x
    for i in range(DEPTH):
        j = i // 2
        if i % 2 == 0:
            h = h + moba_mixer(rms_norm(h, attn_norm[j]), w_qkv[j], q_norm[j], k_norm[j], w_attn_out[j])
            h = h + swiglu(rms_norm(h, ffn_norm[j]), w_ffn_gate_up[j], w_ffn_down[j])
        else:
            h = h + short_conv_mixer(rms_norm(h, conv_norm[j]), w_conv_in[j], conv_w[j], w_conv_out[j])
            h = h + moe_swiglu(rms_norm(h, moe_norm[j]), w_router[j], w_expert_gate_up[j], w_expert_down[j])
    return h
```

```python
import math
from contextlib import ExitStack
import numpy as np
import concourse.bass as bass
import concourse.mybir as mybir
from concourse.bass_utils import run_bass_kernel_spmd

F32 = mybir.dt.float32
BF16 = mybir.dt.bfloat16
AF = mybir.ActivationFunctionType
ALU = mybir.AluOpType
AX = mybir.AxisListType

D = 4096
NKC = 32
SEQ = 2048
HALF = 1024
NH = 32
DFF = 14336
NE = 8
DFE = 5632
EPS = 1e-6
NT = 514
CK = 257
G0 = (1022, 1534)
NQ = 1026
SCALE = 1.0 / math.sqrt(128.0)
BIG2 = 30000.0
DW = 800

C_ONESD, C_ONESH, C_ID, C_D0, C_IOTA, C_PEN, C_BIGM, C_EF = 0, 128, 256, 384, 1408, 1424, 1464, 1504
C_GA, C_GF, C_GC, C_GM, C_QG, C_KG, C_CW, C_WR = 2528, 2560, 2592, 2624, 2656, 2657, 2658, 2754
C_ONEROW = 3010
C_EPS = 3138
NF = 3139
BLKS = (3, 4, 5, 6, 7)

PHASES = 99
CORES = list(range(8))


class DSem:
    def __init__(self, h):
        self.h = h
        self.count = 0


class Tok:
    __slots__ = ("sem", "val", "dma")

    def __init__(self, sem, val, dma=False):
        self.sem, self.val, self.dma = sem, val, dma


class Eng:
    def __init__(self, name, sem, is_pe=False):
        self.name, self.sem, self.n, self.seen, self.is_pe = name, sem, 0, {}, is_pe
        self.prog = []

    def wait(self, toks):
        need = {}
        for t in toks:
            if t is None:
                continue
            if t.dma:
                k, sem, val = id(t.sem), t.sem.h, t.sem.count
            else:
                if self.is_pe and t.sem is self.sem:
                    continue
                k, sem, val = id(t.sem), t.sem, t.val
            if k not in need or need[k][1] < val:
                need[k] = (sem, val)
        for k, (sem, val) in need.items():
            if self.seen.get(k, 0) < val:
                self.prog.append(lambda h, sem=sem, val=val: h.wait_ge(sem, val))
                self.seen[k] = val

    def emit(self, fn):
        self.n += 1
        sem = self.sem
        self.prog.append(lambda h: fn(h).then_inc(sem, 1))
        return Tok(self.sem, self.n)


class Buf:
    def __init__(self, ap, dsem=None):
        self.ap = ap
        self.w = None
        self.r = {}
        self.dsem = dsem

    def add_r(self, t):
        k = id(t.sem)
        if t.dma or k not in self.r or self.r[k].val < t.val:
            self.r[k] = t

    def rdeps(self):
        return [self.w]

    def wdeps(self):
        return [self.w] + list(self.r.values())


def op(eng, fn, reads=(), writes=(), extra=()):
    toks = list(extra)
    for b in reads:
        toks += b.rdeps()
    for b in writes:
        toks += b.wdeps()
    eng.wait(toks)
    t = eng.emit(fn)
    for b in reads:
        b.add_r(t)
    for b in writes:
        b.w = t
        b.r = {}
    return t


def dma(q, out_ap, in_ap, sbuf, load, extra=()):
    toks = list(extra) + (sbuf.wdeps() if load else sbuf.rdeps())
    q.wait(toks)
    semh = sbuf.dsem.h
    q.prog.append(lambda h: h.dma_start(out=out_ap, in_=in_ap).then_inc(semh, 16))
    sbuf.dsem.count += 16
    t = Tok(sbuf.dsem, sbuf.dsem.count, dma=True)
    if load:
        sbuf.w = t
        sbuf.r = {}
    else:
        sbuf.add_r(t)
    return t


def alibi_slope(h):
    return float(np.float32(np.power(2.0, -8.0 * (h + 1) / NH)))


def kcv(ap2d):
    return ap2d.rearrange("(c p) f -> p c f", p=128)


def build_nc():
    nc = bass.Bass("TRN2", target_bir_lowering=False)

    def din(name, shape, dt=F32):
        return nc.dram_tensor(name, shape, dt, kind="ExternalInput").ap()

    xT = din("xT", [D, SEQ])
    w_qkv = din("w_qkv", [D, 3 * D])
    w_ao = din("w_ao", [D, D])
    w_gu = din("w_gu", [D, 2 * DFF])
    w_dn = din("w_dn", [DFF, D])
    w_ci = din("w_ci", [D, 3 * D])
    w_co = din("w_co", [D, D])
    w_egu = din("w_egu", [NE * D, 2 * DFE])
    w_edn = din("w_edn", [NE * DFE, D])
    cF_d = din("cF", [128, NF])
    cB_d = din("cB", [128, 128 + 1024])
    kT = nc.dram_tensor("kT", [NH * 128, SEQ], BF16, kind="Internal").ap()
    qT = nc.dram_tensor("qT", [NH * 128, NQ], F32, kind="Internal").ap()
    vS = nc.dram_tensor("vS", [SEQ, D], BF16, kind="Internal").ap()
    oT = nc.dram_tensor("oT", [D, HALF], F32, kind="ExternalOutput").ap()

    with ExitStack() as es:
        nsem = [0]

        def newsem():
            nsem[0] += 1
            return es.enter_context(nc.semaphore("s%d" % nsem[0]))

        PE = Eng("pe", newsem(), is_pe=True)
        ACT = Eng("act", newsem())
        DVE = Eng("dve", newsem())
        GQ = Eng("gq", newsem())
        SQ = Eng("sq", newsem())
        sempool = {}

        uid = [0]

        def mk(stack, name, shape, dt, dmab=False):
            uid[0] += 1
            t = stack.enter_context(nc.sbuf_tensor("%s_%d" % (name, uid[0]), shape, dt))
            ds = None
            if dmab:
                if name not in sempool:
                    sempool[name] = DSem(newsem())
                ds = sempool[name]
            return Buf(t, ds)

        cF = mk(es, "cF", [128, NF], F32, True)
        cB = mk(es, "cB", [128, 128 + 1024], BF16, True)
        wr_bf = mk(es, "wr_bf", [128, NKC * NE], BF16)
        negB = mk(es, "negB", [128, 1], F32)
        kmean = mk(es, "kmean", [128, NH, 8], F32)
        wbufs = [mk(es, "wb%d" % i, [128, NKC, 256], BF16, True) for i in range(2)]
        pst = es.enter_context(nc.psum_tensor("ps", [128, 8, 512], F32))
        banks = [Buf(None) for _ in range(8)]
        state = {"wi": 0, "slot": 0, "final": []}

        dma(SQ, cF.ap[:], cF_d, cF, True)
        dma(GQ, cB.ap[:], cB_d, cB, True)
        c = cF.ap
        onesD = c[:, C_ONESD:C_ONESD + 128]
        onesH = c[:, C_ONESH:C_ONESH + 128]
        ident = c[:, C_ID:C_ID + 128]
        ones_bf = cB.ap[:, 0:128]

        def E_bf(n):
            return cB.ap[0:8, 128 + n * 128:128 + (n + 1) * 128]

        def E_f(n):
            return c[0:8, C_EF + n * 128:C_EF + (n + 1) * 128]

        op(DVE, lambda h: h.tensor_copy(out=wr_bf.ap[:], in_=c[:, C_WR:C_WR + NKC * NE]), [cF], [wr_bf])

        with ExitStack() as ph:
            ab = mk(ph, "ab", [128, 2], F32)
            mrow = mk(ph, "mrow", [1, 4], F32)
            b7 = banks[7]
            op(ACT, lambda h: h.activation(out=ab.ap[:], in_=c[:, C_QG:C_QG + 2], func=AF.Abs), [cF], [ab])
            for j in range(2):
                op(PE, lambda h, j=j: h.matmul(pst[0:1, 7, 0:128], lhsT=ab.ap[:, j:j + 1], rhs=ident, start=True, stop=True), [ab, cF], [b7])
                op(DVE, lambda h, j=j: h.tensor_reduce(out=mrow.ap[:, j:j + 1], in_=pst[0:1, 7, 0:128], axis=AX.X, op=ALU.max), [b7], [mrow])
            op(DVE, lambda h: h.tensor_scalar(out=mrow.ap[:, 2:3], in0=mrow.ap[:, 0:1], scalar1=mrow.ap[:, 1:2], scalar2=-math.sqrt(128.0),
                                              op0=ALU.mult, op1=ALU.mult), [mrow], [mrow])
            op(PE, lambda h: h.matmul(pst[:, 7, 0:1], lhsT=c[0:1, C_ONEROW:C_ONEROW + 128], rhs=mrow.ap[:, 2:3], start=True, stop=True), [mrow, cF], [b7])
            op(DVE, lambda h: h.tensor_copy(out=negB.ap[:], in_=pst[:, 7, 0:1]), [b7], [negB])

        def wload(pieces, nkc=NKC):
            wb = wbufs[state["wi"] % 2]
            state["wi"] += 1
            for i, (src, coff) in enumerate(pieces):
                ncols = src.shape[1]
                GQ.wait(wb.wdeps() if i == 0 else [])
                semh = wb.dsem.h
                GQ.prog.append(lambda h, src=src, coff=coff, ncols=ncols, semh=semh, wb=wb:
                               h.dma_start(out=wb.ap[:, 0:nkc, coff:coff + ncols], in_=kcv(src)).then_inc(semh, 16))
                wb.dsem.count += 16
            wb.w = Tok(wb.dsem, wb.dsem.count, dma=True)
            wb.r = {}
            return wb

        def slot2():
            s = state["slot"] % 4
            state["slot"] += 1
            return s

        def mm_acc(wb, coff, xbuf, nkc, s, chunks):
            bks = [banks[2 * s + i] for i in range(len(chunks))]

            def fn(h):
                last = None
                for kc in range(nkc):
                    for i, (c0, n) in enumerate(chunks):
                        last = h.matmul(pst[:, 2 * s + i, 0:n], lhsT=wb.ap[:, kc, coff:coff + 128],
                                        rhs=xbuf.ap[:, kc, c0:c0 + n], start=(kc == 0), stop=(kc == nkc - 1))
                return last
            op(PE, fn, [wb, xbuf], bks)
            return bks

        CH2 = [(0, CK), (CK, CK)]

        def barrier():
            toks = [Tok(e.sem, e.n) for e in (PE, ACT, DVE) if e.n > 0]
            toks += [Tok(ds, ds.count, dma=True) for ds in sempool.values() if ds.count > 0]
            for e in (PE, ACT, DVE, SQ):
                e.wait(toks)

        def rmsnorm(src_kc, n, gcol, xn, dcol, sqb, rstd, srcbufs):
            s = slot2()
            bk = banks[2 * s]
            t = None
            for kc in range(NKC):
                q = sqb[kc % 2]
                op(ACT, lambda h, kc=kc, q=q: h.activation(out=q.ap[:, 0:n], in_=src_kc(kc), func=AF.Square), srcbufs, [q])
                t = op(PE, lambda h, kc=kc, q=q: h.matmul(pst[:, 2 * s, 0:n], lhsT=onesD, rhs=q.ap[:, 0:n], start=(kc == 0), stop=(kc == NKC - 1)),
                       [q, cF], [bk] if kc == 0 else [])
            bk.w = t
            op(ACT, lambda h: h.activation(out=rstd.ap[:, 0:n], in_=pst[:, 2 * s, 0:n], func=AF.Sqrt, bias=c[:, C_EPS:C_EPS + 1], scale=1.0), [bk, cF], [rstd])
            op(DVE, lambda h: h.reciprocal(out=rstd.ap[:, 0:n], in_=rstd.ap[:, 0:n]), [rstd], [rstd])
            for kc in range(NKC):
                op(DVE, lambda h, kc=kc: h.scalar_tensor_tensor(out=xn.ap[:, kc, dcol:dcol + n], in0=src_kc(kc), scalar=c[:, gcol + kc:gcol + kc + 1],
                                                                 in1=rstd.ap[:, 0:n], op0=ALU.mult, op1=ALU.mult), srcbufs + [rstd, cF], [xn])

        barrier()
        scratch_w = []
        with ExitStack() as ph:
            xn1 = mk(ph, "xn1", [128, NKC, HALF], BF16)
            xs = mk(ph, "xs", [128, NKC, 256], F32, True)
            sqb = [mk(ph, "sq1_%d" % i, [128, 512], F32) for i in range(2)]
            rstd = mk(ph, "rstd1", [128, 512], F32)
            kf = [mk(ph, "kf%d" % i, [128, 512], F32, True) for i in range(2)]
            kst = [mk(ph, "kst%d" % i, [128, HALF], BF16, True) for i in range(2)]
            vst = [mk(ph, "vst%d" % i, [128, 8, 256], BF16, True) for i in range(2)]
            kfi = [0]

            def qk_head(wb, coff, gcol, chunks, is_k, hd, p):
                s = slot2()
                bks = mm_acc(wb, coff, xn1, NKC, s, chunks)
                ks = kst[hd % 2]
                for i, (c0, n) in enumerate(chunks):
                    s2 = slot2()
                    mb = banks[2 * s2]
                    q = sqb[i % 2]
                    f = kf[kfi[0] % 2]
                    kfi[0] += 1
                    op(ACT, lambda h, i=i, q=q, n=n: h.activation(out=q.ap[:, 0:n], in_=pst[:, 2 * s + i, 0:n], func=AF.Square), [bks[i]], [q])
                    op(PE, lambda h, q=q, s2=s2, n=n: h.matmul(pst[:, 2 * s2, 0:n], lhsT=onesH, rhs=q.ap[:, 0:n], start=True, stop=True), [q, cF], [mb])
                    op(ACT, lambda h, s2=s2, n=n: h.activation(out=rstd.ap[:, 0:n], in_=pst[:, 2 * s2, 0:n], func=AF.Sqrt, bias=c[:, C_EPS:C_EPS + 1], scale=1.0),
                       [mb, cF], [rstd])
                    op(DVE, lambda h, n=n: h.reciprocal(out=rstd.ap[:, 0:n], in_=rstd.ap[:, 0:n]), [rstd], [rstd])
                    op(DVE, lambda h, i=i, n=n, f=f: h.scalar_tensor_tensor(out=f.ap[:, 0:n], in0=pst[:, 2 * s + i, 0:n], scalar=c[:, gcol:gcol + 1],
                                                                             in1=rstd.ap[:, 0:n], op0=ALU.mult, op1=ALU.mult), [bks[i], rstd, cF], [f])
                    if is_k:
                        op(ACT, lambda h, i=i, ks=ks, f=f: h.activation(out=ks.ap[:, i * 512:(i + 1) * 512], in_=f.ap[:], func=AF.Copy), [f], [ks])
                        blk0 = p * 4 + i * 2
                        op(DVE, lambda h, blk0=blk0, f=f: h.tensor_reduce(out=kmean.ap[:, hd, blk0:blk0 + 2],
                                                                           in_=f.ap[:].rearrange("p (b k) -> p b k", b=2), axis=AX.X, op=ALU.add),
                           [f], [kmean])
                    else:
                        qc0 = 0 if p == 0 else 2 + i * 512
                        scratch_w.append(dma(SQ, qT[hd * 128:(hd + 1) * 128, qc0:qc0 + n], f.ap[:, 0:n], f, False))
                if is_k:
                    scratch_w.append(dma(SQ, kT[hd * 128:(hd + 1) * 128, p * HALF:(p + 1) * HALF], ks.ap[:], ks, False))

            for p in range(2):
                t0 = p * HALF
                for cc in range(4):
                    dma(SQ, xs.ap[:], kcv(xT)[:, :, t0 + cc * 256:t0 + (cc + 1) * 256], xs, True)
                    rmsnorm(lambda kc: xs.ap[:, kc, :], 256, C_GA, xn1, cc * 256, sqb, rstd, [xs])
                for hp in range(16):
                    wb = wload([(w_qkv[:, D + hp * 256:D + (hp + 1) * 256], 0)])
                    for hh in range(2):
                        qk_head(wb, hh * 128, C_KG, [(0, 512), (512, 512)], True, 2 * hp + hh, p)
                    wb = wload([(w_qkv[:, hp * 256:(hp + 1) * 256], 0)])
                    for hh in range(2):
                        qk_head(wb, hh * 128, C_QG, [(1022, 2)] if p == 0 else [(0, 512), (512, 512)], False, 2 * hp + hh, p)
                for vb in range(16):
                    wb = wload([(w_qkv[:, 2 * D + vb * 256:2 * D + (vb + 1) * 256], 0)])
                    vs = vst[vb % 2]
                    for tt in range(8):
                        s = slot2()
                        bk = banks[2 * s]

                        def fn(h, tt=tt, s=s, wb=wb):
                            last = None
                            for kc in range(NKC):
                                last = h.matmul(pst[:, 2 * s, 0:256], lhsT=xn1.ap[:, kc, tt * 128:(tt + 1) * 128], rhs=wb.ap[:, kc, :],
                                                start=(kc == 0), stop=(kc == NKC - 1))
                            return last
                        op(PE, fn, [wb, xn1], [bk])
                        op(ACT, lambda h, tt=tt, s=s, vs=vs: h.activation(out=vs.ap[:, tt, :], in_=pst[:, 2 * s, 0:256], func=AF.Copy), [bk], [vs])
                    scratch_w.append(dma(SQ, vS[t0:t0 + HALF, vb * 256:(vb + 1) * 256].rearrange("(t p) c -> p t c", p=128), vs.ap[:], vs, False))

        for gi in range(2):
            g0 = G0[gi]
            barrier()
            with ExitStack() as gs:
                acc = mk(gs, "acc", [128, NKC, NT], F32, True)
                act = mk(gs, "act", [128, NKC, NT], BF16)
                dma(SQ, acc.ap[:], kcv(xT)[:, :, g0:g0 + NT], acc, True)

                def linear_acc(w_ap, xbuf):
                    for blk in range(16):
                        wb = wload([(w_ap[:, blk * 256:(blk + 1) * 256], 0)])
                        for ft in range(2):
                            dm = 2 * blk + ft
                            s = slot2()
                            bks = mm_acc(wb, ft * 128, xbuf, NKC, s, CH2)
                            for i, (c0, n) in enumerate(CH2):
                                op(DVE, lambda h, i=i, c0=c0, n=n, dm=dm, s=s: h.tensor_tensor(out=acc.ap[:, dm, c0:c0 + n], in0=acc.ap[:, dm, c0:c0 + n],
                                                                                                  in1=pst[:, 2 * s + i, 0:n], op=ALU.add), [bks[i], acc], [acc])

                with ExitStack() as ph:
                    kTt = [mk(ph, "kTt%d" % i, [128, SEQ], BF16, True) for i in range(2)]
                    vh = [mk(ph, "vh%d" % i, [128, 16, 128], BF16, True) for i in range(2)]
                    qf = [mk(ph, "qf%d" % i, [128, NT], F32, True) for i in range(2)]
                    qb = mk(ph, "qb", [128, NT], BF16)
                    Dh = mk(ph, "Dh", [128, DW], F32)
                    b16 = mk(ph, "b16", [128, 16], F32)
                    sbb = [mk(ph, "sb%d" % i, [128, CK], F32) for i in range(2)]
                    pT = [mk(ph, "pT%d" % i, [128, CK], BF16) for i in range(2)]
                    selbT = mk(ph, "selbT", [8, NT], BF16)
                    gp = mk(ph, "gp", [128, 8], F32)
                    m8 = mk(ph, "m8", [128, 8], F32)
                    thr = mk(ph, "thr", [128, 1], F32)
                    selb = mk(ph, "selb", [128, 8], F32)
                    rinv = mk(ph, "rinv", [128, CK], F32)
                    qtiles = [(0, 2, g0 // 256)] + [(2 + 128 * i, 128, (g0 + 2 + 128 * i) // 256) for i in range(4)]
                    it = [0]
                    for hd in range(NH):
                        sl = hd % 2
                        m_h = alibi_slope(hd)
                        nkeys = g0 + NT
                        nkt_all = (nkeys + 127) // 128
                        dma(SQ, kTt[sl].ap[:, 0:nkt_all * 128], kT[hd * 128:(hd + 1) * 128, 0:nkt_all * 128], kTt[sl], True, extra=scratch_w)
                        dma(SQ, vh[sl].ap[:, 0:nkt_all, :], vS[0:nkt_all * 128, hd * 128:(hd + 1) * 128].rearrange("(t p) d -> p t d", p=128),
                            vh[sl], True, extra=scratch_w)
                        dma(SQ, qf[sl].ap[:], qT[hd * 128:(hd + 1) * 128, gi * 512:gi * 512 + NT], qf[sl], True, extra=scratch_w)
                        op(ACT, lambda h, sl=sl: h.activation(out=qb.ap[:], in_=qf[sl].ap[:], func=AF.Copy), [qf[sl]], [qb])
                        op(DVE, lambda h, m_h=m_h: h.tensor_scalar(out=Dh.ap[:], in0=c[:, C_D0:C_D0 + DW], scalar1=-m_h, scalar2=None, op0=ALU.mult),
                           [cF], [Dh])
                        op(DVE, lambda h, m_h=m_h: h.tensor_scalar(out=b16.ap[:], in0=c[:, C_IOTA:C_IOTA + 16], scalar1=-128.0 * m_h, scalar2=negB.ap[:, 0:1],
                                                                   op0=ALU.mult, op1=ALU.add), [cF, negB], [b16])
                        b7 = banks[7]
                        for (c0, nq, blk) in qtiles:
                            bi = BLKS.index(blk)
                            op(PE, lambda h, c0=c0, nq=nq, sl=sl, hd=hd: h.matmul(pst[0:nq, 7, 0:8], lhsT=qf[sl].ap[:, c0:c0 + nq], rhs=kmean.ap[:, hd, :],
                                                                                   start=True, stop=True), [qf[sl], kmean], [b7])
                            op(DVE, lambda h, nq=nq, bi=bi: h.tensor_tensor(out=gp.ap[0:nq, :], in0=pst[0:nq, 7, 0:8], in1=c[0:nq, C_PEN + 8 * bi:C_PEN + 8 * bi + 8],
                                                                            op=ALU.add), [b7, cF], [gp])
                            op(DVE, lambda h, nq=nq: h.max(out=m8.ap[0:nq, :], in_=gp.ap[0:nq, :]), [gp], [m8])
                            op(DVE, lambda h, nq=nq: h.tensor_scalar(out=thr.ap[0:nq, :], in0=m8.ap[0:nq, 2:3], scalar1=-5.0e8, scalar2=None, op0=ALU.max),
                               [m8], [thr])
                            op(DVE, lambda h, nq=nq: h.tensor_scalar(out=selb.ap[0:nq, :], in0=gp.ap[0:nq, :], scalar1=thr.ap[0:nq, 0:1], scalar2=None,
                                                                     op0=ALU.is_ge), [gp, thr], [selb])
                            op(DVE, lambda h, nq=nq, bi=bi: h.scalar_tensor_tensor(out=selb.ap[0:nq, :], in0=selb.ap[0:nq, :], scalar=-1.0,
                                                                                   in1=c[0:nq, C_BIGM + 8 * bi:C_BIGM + 8 * bi + 8], op0=ALU.add, op1=ALU.mult),
                               [selb, cF], [selb])
                            op(PE, lambda h, nq=nq: h.matmul(pst[0:8, 7, 16:16 + nq], lhsT=selb.ap[0:nq, :], rhs=c[0:nq, C_ID:C_ID + nq], start=True, stop=True),
                               [selb, cF], [b7])
                            op(ACT, lambda h, c0=c0, nq=nq: h.activation(out=selbT.ap[:, c0:c0 + nq], in_=pst[0:8, 7, 16:16 + nq], func=AF.Copy), [b7], [selbT])
                        for ck in range(2):
                            cc0 = ck * CK
                            q0 = g0 + cc0
                            nkt = (q0 + CK - 1) // 128 + 1
                            ob, sb_ = banks[3], banks[4]
                            tO = tS = None
                            for kt in range(nkt):
                                k0 = kt * 128
                                sbk = banks[kt % 3]
                                delta = q0 - k0
                                a = max(0, -(-(delta - 128) // 128))
                                dp = delta - 128 * a
                                coff = dp + 384
                                assert 0 <= coff and coff + CK <= DW, (coff, delta)
                                ii = it[0] % 2
                                it[0] += 1

                                def fs(h, kt=kt, k0=k0, sl=sl, cc0=cc0):
                                    h.matmul(pst[:, kt % 3, 0:CK], lhsT=kTt[sl].ap[:, k0:k0 + 128], rhs=qb.ap[:, cc0:cc0 + CK], start=True, stop=False)
                                    return h.matmul(pst[:, kt % 3, 0:CK], lhsT=E_bf(kt // 2), rhs=selbT.ap[:, cc0:cc0 + CK], start=False, stop=True)
                                op(PE, fs, [kTt[sl], qb, selbT, cB], [sbk])
                                op(DVE, lambda h, kt=kt, coff=coff, ii=ii: h.scalar_tensor_tensor(out=sbb[ii].ap[:], in0=pst[:, kt % 3, 0:CK], scalar=SCALE,
                                                                                                   in1=Dh.ap[:, coff:coff + CK], op0=ALU.mult, op1=ALU.add),
                                   [sbk, Dh], [sbb[ii]])
                                op(ACT, lambda h, ii=ii, a=a: h.activation(out=pT[ii].ap[:], in_=sbb[ii].ap[:], func=AF.Exp, bias=b16.ap[:, a:a + 1], scale=1.0),
                                   [sbb[ii], b16], [pT[ii]])
                                tO = op(PE, lambda h, kt=kt, sl=sl, ii=ii, nkt=nkt: h.matmul(pst[:, 3, 0:CK], lhsT=vh[sl].ap[:, kt, :], rhs=pT[ii].ap[:],
                                                                                             start=(kt == 0), stop=(kt == nkt - 1)),
                                        [vh[sl], pT[ii]], [ob] if kt == 0 else [])
                                tS = op(PE, lambda h, kt=kt, ii=ii, nkt=nkt: h.matmul(pst[:, 4, 0:CK], lhsT=ones_bf, rhs=pT[ii].ap[:],
                                                                                      start=(kt == 0), stop=(kt == nkt - 1)),
                                        [pT[ii], cB], [sb_] if kt == 0 else [])
                            ob.w, sb_.w = tO, tS
                            op(DVE, lambda h: h.reciprocal(out=rinv.ap[:], in_=pst[:, 4, 0:CK]), [sb_], [rinv])
                            op(DVE, lambda h, hd=hd, cc0=cc0: h.tensor_tensor(out=act.ap[:, hd, cc0:cc0 + CK], in0=pst[:, 3, 0:CK], in1=rinv.ap[:], op=ALU.mult),
                               [ob, rinv], [act])

                barrier()
                linear_acc(w_ao, act)

                def store_out(skip_wait=False):
                    t = dma(SQ, kcv(oT)[:, :, gi * 512:(gi + 1) * 512], acc.ap[:, :, 2:NT], acc, False)
                    state["final"].append(t)

                if PHASES <= 3:
                    store_out()
                    continue

                with ExitStack() as ph:
                    xn = mk(ph, "xn", [128, NKC, NT], BF16)
                    sq = [mk(ph, "sq%d" % i, [128, CK], F32) for i in range(2)]
                    rstd_g = mk(ph, "rstd", [128, CK], F32)
                    sg = [mk(ph, "sg%d" % i, [128, CK], F32) for i in range(2)]
                    cwbc = mk(ph, "cwbc", [128, NT], F32)
                    combT = mk(ph, "combT", [8, NT], F32)
                    ub = mk(ph, "ub", [128, NT], F32)
                    yb = mk(ph, "yb", [128, NT], F32)
                    ctb = [mk(ph, "ct%d" % i, [128, CK], F32) for i in range(2)]
                    lg = mk(ph, "lg", [128, 8], F32)
                    ex = mk(ph, "ex", [128, 8], F32)
                    m8r = mk(ph, "m8b", [128, 8], F32)
                    nv1 = mk(ph, "nv1", [128, 1], F32)
                    den = mk(ph, "den", [128, 1], F32)
                    sgi = [0]

                    def norm(gcol):
                        for (c0, n) in CH2:
                            rmsnorm(lambda kc, c0=c0, n=n: acc.ap[:, kc, c0:c0 + n], n, gcol, xn, c0, sq, rstd_g, [acc])

                    def mlp_slice(w_gu_ap, gcol0, ucol0, nft, w_dn_ap, cw):
                        for j in range(nft):
                            wb = wload([(w_gu_ap[:, gcol0 + j * 128:gcol0 + (j + 1) * 128], 0), (w_gu_ap[:, ucol0 + j * 128:ucol0 + (j + 1) * 128], 128)])
                            sgs = slot2()
                            gb = mm_acc(wb, 0, xn, NKC, sgs, CH2)
                            sus = slot2()
                            ubk = mm_acc(wb, 128, xn, NKC, sus, CH2)
                            for i, (c0, n) in enumerate(CH2):
                                g = sg[sgi[0] % 2]
                                sgi[0] += 1
                                op(ACT, lambda h, i=i, g=g, sgs=sgs: h.activation(out=g.ap[:], in_=pst[:, 2 * sgs + i, 0:CK], func=AF.Silu), [gb[i]], [g])
                                if cw is not None:
                                    op(DVE, lambda h, g=g, c0=c0: h.tensor_tensor(out=g.ap[:], in0=g.ap[:], in1=cw.ap[:, c0:c0 + CK], op=ALU.mult), [g, cw], [g])
                                op(DVE, lambda h, i=i, g=g, j=j, c0=c0, sus=sus: h.tensor_tensor(out=act.ap[:, j, c0:c0 + CK], in0=g.ap[:], in1=pst[:, 2 * sus + i, 0:CK],
                                                                                                   op=ALU.mult), [g, ubk[i]], [act])
                        for blk in range(16):
                            wb = wload([(w_dn_ap[:, blk * 256:(blk + 1) * 256], 0)], nkc=nft)
                            for ft in range(2):
                                dm = 2 * blk + ft
                                s = slot2()
                                bks = mm_acc(wb, ft * 128, act, nft, s, CH2)
                                for i, (c0, n) in enumerate(CH2):
                                    op(DVE, lambda h, i=i, c0=c0, n=n, dm=dm, s=s: h.tensor_tensor(out=acc.ap[:, dm, c0:c0 + n], in0=acc.ap[:, dm, c0:c0 + n],
                                                                                                      in1=pst[:, 2 * s + i, 0:n], op=ALU.add), [bks[i], acc], [acc])

                    norm(C_GF)
                    for sidx in range(4):
                        mlp_slice(w_gu, sidx * 3584, DFF + sidx * 3584, 28, w_dn[sidx * 3584:(sidx + 1) * 3584, :], None)
                    if PHASES <= 4:
                        store_out()
                        continue

                    norm(C_GC)
                    for j in range(NKC):
                        wb = wload([(w_ci[:, D + j * 128:D + (j + 1) * 128], 0), (w_ci[:, 2 * D + j * 128:2 * D + (j + 1) * 128], 128)])
                        scs = slot2()
                        cb = mm_acc(wb, 0, xn, NKC, scs, CH2)
                        shs = slot2()
                        hb = mm_acc(wb, 128, xn, NKC, shs, CH2)
                        wb2 = wload([(w_ci[:, j * 128:(j + 1) * 128], 0)])
                        sbs = slot2()
                        bb = mm_acc(wb2, 0, xn, NKC, sbs, CH2)
                        for i, (c0, n) in enumerate(CH2):
                            ct = ctb[i]
                            op(ACT, lambda h, i=i, ct=ct, scs=scs: h.activation(out=ct.ap[:], in_=pst[:, 2 * scs + i, 0:CK], func=AF.Copy), [cb[i]], [ct])
                            op(DVE, lambda h, i=i, ct=ct, c0=c0, shs=shs: h.tensor_tensor(out=ub.ap[:, c0:c0 + CK], in0=ct.ap[:], in1=pst[:, 2 * shs + i, 0:CK], op=ALU.mult),
                               [ct, hb[i]], [ub])
                        cwc = C_CW
                        op(DVE, lambda h, j=j: h.tensor_scalar(out=yb.ap[:], in0=ub.ap[:], scalar1=c[:, cwc + 64 + j:cwc + 65 + j], scalar2=None, op0=ALU.mult), [ub, cF], [yb])
                        op(DVE, lambda h, j=j: h.scalar_tensor_tensor(out=yb.ap[:, 1:NT], in0=ub.ap[:, 0:NT - 1], scalar=c[:, cwc + 32 + j:cwc + 33 + j], in1=yb.ap[:, 1:NT],
                                                                       op0=ALU.mult, op1=ALU.add), [ub, cF, yb], [yb])
                        op(DVE, lambda h, j=j: h.scalar_tensor_tensor(out=yb.ap[:, 2:NT], in0=ub.ap[:, 0:NT - 2], scalar=c[:, cwc + j:cwc + 1 + j], in1=yb.ap[:, 2:NT],
                                                                       op0=ALU.mult, op1=ALU.add), [ub, cF, yb], [yb])
                        for i, (c0, n) in enumerate(CH2):
                            op(DVE, lambda h, i=i, j=j, c0=c0, sbs=sbs: h.tensor_tensor(out=act.ap[:, j, c0:c0 + CK], in0=yb.ap[:, c0:c0 + CK], in1=pst[:, 2 * sbs + i, 0:CK],
                                                                                        op=ALU.mult), [yb, bb[i]], [act])
                    linear_acc(w_co, act)
                    if PHASES <= 6:
                        store_out()
                        continue

                    norm(C_GM)
                    op(DVE, lambda h: h.memset(combT.ap[:, 0:2], 0.0), [], [combT])
                    for i4 in range(4):
                        c0 = 2 + 128 * i4
                        s = slot2()
                        bk = banks[2 * s]

                        def fr(h, c0=c0, s=s):
                            last = None
                            for kc in range(NKC):
                                last = h.matmul(pst[:, 2 * s, 0:8], lhsT=xn.ap[:, kc, c0:c0 + 128], rhs=wr_bf.ap[:, kc * NE:(kc + 1) * NE],
                                                start=(kc == 0), stop=(kc == NKC - 1))
                            return last
                        op(PE, fr, [xn, wr_bf], [bk])
                        op(DVE, lambda h, s=s: h.tensor_copy(out=lg.ap[:], in_=pst[:, 2 * s, 0:8]), [bk], [lg])
                        op(DVE, lambda h: h.max(out=m8r.ap[:], in_=lg.ap[:]), [lg], [m8r])
                        op(DVE, lambda h: h.tensor_scalar(out=nv1.ap[:], in0=m8r.ap[:, 0:1], scalar1=-1.0, scalar2=None, op0=ALU.mult), [m8r], [nv1])
                        op(ACT, lambda h: h.activation(out=ex.ap[:], in_=lg.ap[:], func=AF.Exp, bias=nv1.ap[:, 0:1], scale=1.0), [lg, nv1], [ex])
                        op(DVE, lambda h: h.tensor_scalar(out=lg.ap[:], in0=lg.ap[:], scalar1=m8r.ap[:, 1:2], scalar2=None, op0=ALU.is_ge), [lg, m8r, ex], [lg])
                        op(DVE, lambda h: h.tensor_tensor(out=ex.ap[:], in0=ex.ap[:], in1=lg.ap[:], op=ALU.mult), [lg, ex], [ex])
                        op(DVE, lambda h: h.tensor_reduce(out=den.ap[:], in_=ex.ap[:], axis=AX.X, op=ALU.add), [ex], [den])
                        op(DVE, lambda h: h.reciprocal(out=den.ap[:], in_=den.ap[:]), [den], [den])
                        op(DVE, lambda h: h.tensor_scalar(out=ex.ap[:], in0=ex.ap[:], scalar1=den.ap[:, 0:1], scalar2=None, op0=ALU.mult), [ex, den], [ex])
                        op(PE, lambda h, s=s: h.matmul(pst[0:8, 2 * s + 1, 0:128], lhsT=ex.ap[:], rhs=ident, start=True, stop=True), [ex, cF], [banks[2 * s + 1]])
                        op(ACT, lambda h, s=s, c0=c0: h.activation(out=combT.ap[:, c0:c0 + 128], in_=pst[0:8, 2 * s + 1, 0:128], func=AF.Copy),
                           [banks[2 * s + 1]], [combT])
                    for e in range(NE):
                        s = slot2()
                        bks = [banks[2 * s], banks[2 * s + 1]]
                        for i, (c0, n) in enumerate(CH2):
                            op(PE, lambda h, i=i, c0=c0, e=e, s=s: h.matmul(pst[:, 2 * s + i, 0:CK], lhsT=E_f(e), rhs=combT.ap[:, c0:c0 + CK], start=True, stop=True),
                               [combT, cF], [bks[i]])
                            op(ACT, lambda h, i=i, c0=c0, s=s: h.activation(out=cwbc.ap[:, c0:c0 + CK], in_=pst[:, 2 * s + i, 0:CK], func=AF.Copy), [bks[i]], [cwbc])
                        for hf in range(2):
                            mlp_slice(w_egu[e * D:(e + 1) * D, :], hf * 2816, DFE + hf * 2816, 22,
                                      w_edn[e * DFE + hf * 2816:e * DFE + (hf + 1) * 2816, :], cwbc)
                    store_out()

        SQ.wait(state["final"])
        engs = {"tensor": PE, "scalar": ACT, "vector": DVE, "gpsimd": GQ, "sync": SQ}
        with nc.Block() as block:
            for name, e in engs.items():
                def body(h, e=e):
                    for f in e.prog:
                        f(h)
                getattr(block, name)(body)
    return nc


def make_consts(core, inputs):
    cf = np.zeros((128, NF), np.float32)
    cf[:, C_ONESD:C_ONESD + 128] = 1.0 / D
    cf[:, C_ONESH:C_ONESH + 128] = 1.0 / 128.0
    cf[:, C_ID:C_ID + 128] = np.eye(128, dtype=np.float32)
    p = np.arange(128)[:, None]
    cc = np.arange(1024)[None, :]
    dist = (cc - p - 384).astype(np.float32)
    cf[:, C_D0:C_D0 + 1024] = np.where(dist >= 0, dist, np.float32(1e34))
    cf[:, C_IOTA:C_IOTA + 16] = np.arange(16, dtype=np.float32)[None, :]
    pen_core = -1.0e9 if core % 2 == 0 else 0.0
    for bi, blk in enumerate(BLKS):
        n = np.arange(8)
        pen = np.where(n < 4, pen_core, 0.0) + np.where(n >= blk, -1.0e9, 0.0)
        cf[:, C_PEN + 8 * bi:C_PEN + 8 * bi + 8] = pen[None, :]
        cf[:, C_BIGM + 8 * bi:C_BIGM + 8 * bi + 8] = np.where(n == blk, 0.0, BIG2)[None, :]
    for n in range(8):
        cf[n, C_EF + n * 128:C_EF + (n + 1) * 128] = 1.0
    for col, key in ((C_GA, "attn_norm"), (C_GF, "ffn_norm"), (C_GC, "conv_norm"), (C_GM, "moe_norm")):
        cf[:, col:col + 32] = inputs[key][0].reshape(32, 128).T
    cf[:, C_QG] = inputs["q_norm"][0]
    cf[:, C_KG] = inputs["k_norm"][0]
    cw = inputs["conv_w"][0]
    for k in range(3):
        cf[:, C_CW + 32 * k:C_CW + 32 * k + 32] = cw[k].reshape(32, 128).T
    cf[:, C_WR:C_WR + 256] = inputs["w_router"][0].reshape(32, 128, 8).transpose(1, 0, 2).reshape(128, 256)
    cf[:, C_ONEROW:C_ONEROW + 128] = 1.0
    cf[:, C_EPS] = EPS
    cb = np.zeros((128, 128 + 1024), np.float32)
    cb[:, 0:128] = 1.0
    for n in range(8):
        cb[n, 128 + n * 128:128 + (n + 1) * 128] = 1.0
    return cf, cb


_NC_CACHE = {}


def kernel(**inputs):
    inputs = {k: np.asarray(v) for k, v in inputs.items()}
    x = inputs["x"]
    if "nc" not in _NC_CACHE:
        _NC_CACHE["nc"] = build_nc()
    nc = _NC_CACHE["nc"]
    shared = {
        "w_qkv": inputs["w_qkv"][0], "w_ao": inputs["w_attn_out"][0], "w_gu": inputs["w_ffn_gate_up"][0],
        "w_dn": inputs["w_ffn_down"][0], "w_ci": inputs["w_conv_in"][0], "w_co": inputs["w_conv_out"][0],
        "w_egu": inputs["w_expert_gate_up"][0].reshape(NE * D, 2 * DFE), "w_edn": inputs["w_expert_down"][0].reshape(NE * DFE, D),
    }
    in_maps = []
    for core in CORES:
        b, hf = core // 2, core % 2
        xw = np.zeros((D, SEQ), np.float32)
        if hf == 1:
            xw[:, :] = x[b].T
        else:
            xw[:, HALF:] = x[b, :HALF].T
        cf, cb = make_consts(core, inputs)
        m = dict(shared)
        m.update({"xT": xw, "cF": cf, "cB": cb})
        in_maps.append(m)
    res = run_bass_kernel_spmd(nc, in_maps, core_ids=list(range(len(CORES))))
    out = np.zeros_like(x)
    for i, core in enumerate(CORES):
        b, hf = core // 2, core % 2
        out[b, hf * HALF:(hf + 1) * HALF, :] = res.results[i]["oT"].T
    return out
```

```python
import math
from contextlib import ExitStack
import numpy as np
import concourse.bass as bass
import concourse.mybir as mybir
from concourse.bass_utils import run_bass_kernel_spmd

F32 = mybir.dt.float32
BF16 = mybir.dt.bfloat16
AF = mybir.ActivationFunctionType
ALU = mybir.AluOpType
AX = mybir.AxisListType

D = 4096
NKC = 32
SEQ = 2048
HALF = 1024
NH = 32
DFF = 14336
NE = 8
DFE = 5632
EPS = 1e-6
NT = 514
CK = 257
G0 = (1022, 1534)
NQ = 1026
SCALE = 1.0 / math.sqrt(128.0)
BIG2 = 30000.0
DW = 800

C_ONESD, C_ONESH, C_ID, C_D0, C_IOTA, C_PEN, C_BIGM, C_EF = 0, 128, 256, 384, 1408, 1424, 1464, 1504
C_GA, C_GF, C_GC, C_GM, C_QG, C_KG, C_CW, C_WR = 2528, 2560, 2592, 2624, 2656, 2657, 2658, 2754
C_ONEROW = 3010
C_EPS = 3138
NF = 3139
BLKS = (3, 4, 5, 6, 7)

PHASES = 99
CORES = list(range(8))


class DSem:
    def __init__(self, h):
        self.h = h
        self.count = 0


class Tok:
    __slots__ = ("sem", "val", "dma")

    def __init__(self, sem, val, dma=False):
        self.sem, self.val, self.dma = sem, val, dma


class Eng:
    def __init__(self, name, sem, is_pe=False):
        self.name, self.sem, self.n, self.seen, self.is_pe = name, sem, 0, {}, is_pe
        self.prog = []

    def wait(self, toks):
        need = {}
        for t in toks:
            if t is None:
                continue
            if t.dma:
                k, sem, val = id(t.sem), t.sem.h, t.sem.count
            else:
                if self.is_pe and t.sem is self.sem:
                    continue
                k, sem, val = id(t.sem), t.sem, t.val
            if k not in need or need[k][1] < val:
                need[k] = (sem, val)
        for k, (sem, val) in need.items():
            if self.seen.get(k, 0) < val:
                self.prog.append(lambda h, sem=sem, val=val: h.wait_ge(sem, val))
                self.seen[k] = val

    def emit(self, fn):
        self.n += 1
        sem = self.sem
        self.prog.append(lambda h: fn(h).then_inc(sem, 1))
        return Tok(self.sem, self.n)


class Buf:
    def __init__(self, ap, dsem=None):
        self.ap = ap
        self.w = None
        self.r = {}
        self.dsem = dsem

    def add_r(self, t):
        k = id(t.sem)
        if t.dma or k not in self.r or self.r[k].val < t.val:
            self.r[k] = t

    def rdeps(self):
        return [self.w]

    def wdeps(self):
        return [self.w] + list(self.r.values())


def op(eng, fn, reads=(), writes=(), extra=()):
    toks = list(extra)
    for b in reads:
        toks += b.rdeps()
    for b in writes:
        toks += b.wdeps()
    eng.wait(toks)
    t = eng.emit(fn)
    for b in reads:
        b.add_r(t)
    for b in writes:
        b.w = t
        b.r = {}
    return t


def dma(q, out_ap, in_ap, sbuf, load, extra=()):
    toks = list(extra) + (sbuf.wdeps() if load else sbuf.rdeps())
    q.wait(toks)
    semh = sbuf.dsem.h
    q.prog.append(lambda h: h.dma_start(out=out_ap, in_=in_ap).then_inc(semh, 16))
    sbuf.dsem.count += 16
    t = Tok(sbuf.dsem, sbuf.dsem.count, dma=True)
    if load:
        sbuf.w = t
        sbuf.r = {}
    else:
        sbuf.add_r(t)
    return t


def alibi_slope(h):
    return float(np.float32(np.power(2.0, -8.0 * (h + 1) / NH)))


def kcv(ap2d):
    return ap2d.rearrange("(c p) f -> p c f", p=128)


def build_nc():
    nc = bass.Bass("TRN2", target_bir_lowering=False)

    def din(name, shape, dt=F32):
        return nc.dram_tensor(name, shape, dt, kind="ExternalInput").ap()

    xT = din("xT", [D, SEQ])
    w_qkv = din("w_qkv", [D, 3 * D])
    w_ao = din("w_ao", [D, D])
    w_gu = din("w_gu", [D, 2 * DFF])
    w_dn = din("w_dn", [DFF, D])
    w_ci = din("w_ci", [D, 3 * D])
    w_co = din("w_co", [D, D])
    w_egu = din("w_egu", [NE * D, 2 * DFE])
    w_edn = din("w_edn", [NE * DFE, D])
    cF_d = din("cF", [128, NF])
    cB_d = din("cB", [128, 128 + 1024])
    kT = nc.dram_tensor("kT", [NH * 128, SEQ], BF16, kind="Internal").ap()
    qT = nc.dram_tensor("qT", [NH * 128, NQ], F32, kind="Internal").ap()
    vS = nc.dram_tensor("vS", [SEQ, D], BF16, kind="Internal").ap()
    oT = nc.dram_tensor("oT", [D, HALF], F32, kind="ExternalOutput").ap()

    with ExitStack() as es:
        nsem = [0]

        def newsem():
            nsem[0] += 1
            return es.enter_context(nc.semaphore("s%d" % nsem[0]))

        PE = Eng("pe", newsem(), is_pe=True)
        ACT = Eng("act", newsem())
        DVE = Eng("dve", newsem())
        GQ = Eng("gq", newsem())
        SQ = Eng("sq", newsem())
        sempool = {}

        uid = [0]

        def mk(stack, name, shape, dt, dmab=False):
            uid[0] += 1
            t = stack.enter_context(nc.sbuf_tensor("%s_%d" % (name, uid[0]), shape, dt))
            ds = None
            if dmab:
                if name not in sempool:
                    sempool[name] = DSem(newsem())
                ds = sempool[name]
            return Buf(t, ds)

        cF = mk(es, "cF", [128, NF], F32, True)
        cB = mk(es, "cB", [128, 128 + 1024], BF16, True)
        wr_bf = mk(es, "wr_bf", [128, NKC * NE], BF16)
        negB = mk(es, "negB", [128, 1], F32)
        kmean = mk(es, "kmean", [128, NH, 8], F32)
        wbufs = [mk(es, "wb%d" % i, [128, NKC, 256], BF16, True) for i in range(3)]
        pst = es.enter_context(nc.psum_tensor("ps", [128, 8, 512], F32))
        banks = [Buf(None) for _ in range(8)]
        state = {"wi": 0, "slot": 0, "final": []}

        dma(SQ, cF.ap[:], cF_d, cF, True)
        dma(GQ, cB.ap[:], cB_d, cB, True)
        c = cF.ap
        onesD = c[:, C_ONESD:C_ONESD + 128]
        onesH = c[:, C_ONESH:C_ONESH + 128]
        ident = c[:, C_ID:C_ID + 128]
        ones_bf = cB.ap[:, 0:128]

        def E_bf(n):
            return cB.ap[0:8, 128 + n * 128:128 + (n + 1) * 128]

        def E_f(n):
            return c[0:8, C_EF + n * 128:C_EF + (n + 1) * 128]

        op(DVE, lambda h: h.tensor_copy(out=wr_bf.ap[:], in_=c[:, C_WR:C_WR + NKC * NE]), [cF], [wr_bf])

        with ExitStack() as ph:
            ab = mk(ph, "ab", [128, 2], F32)
            mrow = mk(ph, "mrow", [1, 4], F32)
            b7 = banks[7]
            op(ACT, lambda h: h.activation(out=ab.ap[:], in_=c[:, C_QG:C_QG + 2], func=AF.Abs), [cF], [ab])
            for j in range(2):
                op(PE, lambda h, j=j: h.matmul(pst[0:1, 7, 0:128], lhsT=ab.ap[:, j:j + 1], rhs=ident, start=True, stop=True), [ab, cF], [b7])
                op(DVE, lambda h, j=j: h.tensor_reduce(out=mrow.ap[:, j:j + 1], in_=pst[0:1, 7, 0:128], axis=AX.X, op=ALU.max), [b7], [mrow])
            op(DVE, lambda h: h.tensor_scalar(out=mrow.ap[:, 2:3], in0=mrow.ap[:, 0:1], scalar1=mrow.ap[:, 1:2], scalar2=-math.sqrt(128.0),
                                              op0=ALU.mult, op1=ALU.mult), [mrow], [mrow])
            op(PE, lambda h: h.matmul(pst[:, 7, 0:1], lhsT=c[0:1, C_ONEROW:C_ONEROW + 128], rhs=mrow.ap[:, 2:3], start=True, stop=True), [mrow, cF], [b7])
            op(DVE, lambda h: h.tensor_copy(out=negB.ap[:], in_=pst[:, 7, 0:1]), [b7], [negB])

        def wload(pieces, nkc=NKC):
            wb = wbufs[state["wi"] % 3]
            state["wi"] += 1
            for i, (src, coff) in enumerate(pieces):
                ncols = src.shape[1]
                GQ.wait(wb.wdeps() if i == 0 else [])
                semh = wb.dsem.h
                GQ.prog.append(lambda h, src=src, coff=coff, ncols=ncols, semh=semh, wb=wb:
                               h.dma_start(out=wb.ap[:, 0:nkc, coff:coff + ncols], in_=kcv(src)).then_inc(semh, 16))
                wb.dsem.count += 16
            wb.w = Tok(wb.dsem, wb.dsem.count, dma=True)
            wb.r = {}
            return wb

        def slot2():
            s = state["slot"] % 4
            state["slot"] += 1
            return s

        def mm_acc(wb, coff, xbuf, nkc, s, chunks):
            bks = [banks[2 * s + i] for i in range(len(chunks))]

            def fn(h):
                last = None
                for kc in range(nkc):
                    for i, (c0, n) in enumerate(chunks):
                        last = h.matmul(pst[:, 2 * s + i, 0:n], lhsT=wb.ap[:, kc, coff:coff + 128],
                                        rhs=xbuf.ap[:, kc, c0:c0 + n], start=(kc == 0), stop=(kc == nkc - 1))
                return last
            op(PE, fn, [wb, xbuf], bks)
            return bks

        CH2 = [(0, CK), (CK, CK)]

        def barrier():
            toks = [Tok(e.sem, e.n) for e in (PE, ACT, DVE) if e.n > 0]
            toks += [Tok(ds, ds.count, dma=True) for ds in sempool.values() if ds.count > 0]
            for e in (PE, ACT, DVE, SQ):
                e.wait(toks)

        def rmsnorm(src_kc, n, gcol, xn, dcol, sqb, rstd, srcbufs):
            s = slot2()
            bk = banks[2 * s]
            t = None
            for kc in range(NKC):
                q = sqb[kc % 2]
                op(ACT, lambda h, kc=kc, q=q: h.activation(out=q.ap[:, 0:n], in_=src_kc(kc), func=AF.Square), srcbufs, [q])
                t = op(PE, lambda h, kc=kc, q=q: h.matmul(pst[:, 2 * s, 0:n], lhsT=onesD, rhs=q.ap[:, 0:n], start=(kc == 0), stop=(kc == NKC - 1)),
                       [q, cF], [bk] if kc == 0 else [])
            bk.w = t
            op(ACT, lambda h: h.activation(out=rstd.ap[:, 0:n], in_=pst[:, 2 * s, 0:n], func=AF.Sqrt, bias=c[:, C_EPS:C_EPS + 1], scale=1.0), [bk, cF], [rstd])
            op(DVE, lambda h: h.reciprocal(out=rstd.ap[:, 0:n], in_=rstd.ap[:, 0:n]), [rstd], [rstd])
            for kc in range(NKC):
                op(DVE, lambda h, kc=kc: h.scalar_tensor_tensor(out=xn.ap[:, kc, dcol:dcol + n], in0=src_kc(kc), scalar=c[:, gcol + kc:gcol + kc + 1],
                                                                 in1=rstd.ap[:, 0:n], op0=ALU.mult, op1=ALU.mult), srcbufs + [rstd, cF], [xn])

        barrier()
        scratch_w = []
        with ExitStack() as ph:
            xn1 = mk(ph, "xn1", [128, NKC, HALF], BF16)
            xs = mk(ph, "xs", [128, NKC, 256], F32, True)
            sqb = [mk(ph, "sq1_%d" % i, [128, 512], F32) for i in range(2)]
            rstd = mk(ph, "rstd1", [128, 512], F32)
            kf = [mk(ph, "kf%d" % i, [128, 512], F32, True) for i in range(2)]
            kst = [mk(ph, "kst%d" % i, [128, HALF], BF16, True) for i in range(2)]
            vst = [mk(ph, "vst%d" % i, [128, 8, 256], BF16, True) for i in range(2)]
            kfi = [0]

            def qk_head(wb, coff, gcol, chunks, is_k, hd, p):
                s = slot2()
                bks = mm_acc(wb, coff, xn1, NKC, s, chunks)
                ks = kst[hd % 2]
                for i, (c0, n) in enumerate(chunks):
                    s2 = slot2()
                    mb = banks[2 * s2]
                    q = sqb[i % 2]
                    f = kf[kfi[0] % 2]
                    kfi[0] += 1
                    op(ACT, lambda h, i=i, q=q, n=n: h.activation(out=q.ap[:, 0:n], in_=pst[:, 2 * s + i, 0:n], func=AF.Square), [bks[i]], [q])
                    op(PE, lambda h, q=q, s2=s2, n=n: h.matmul(pst[:, 2 * s2, 0:n], lhsT=onesH, rhs=q.ap[:, 0:n], start=True, stop=True), [q, cF], [mb])
                    op(ACT, lambda h, s2=s2, n=n: h.activation(out=rstd.ap[:, 0:n], in_=pst[:, 2 * s2, 0:n], func=AF.Sqrt, bias=c[:, C_EPS:C_EPS + 1], scale=1.0),
                       [mb, cF], [rstd])
                    op(DVE, lambda h, n=n: h.reciprocal(out=rstd.ap[:, 0:n], in_=rstd.ap[:, 0:n]), [rstd], [rstd])
                    op(DVE, lambda h, i=i, n=n, f=f: h.scalar_tensor_tensor(out=f.ap[:, 0:n], in0=pst[:, 2 * s + i, 0:n], scalar=c[:, gcol:gcol + 1],
                                                                             in1=rstd.ap[:, 0:n], op0=ALU.mult, op1=ALU.mult), [bks[i], rstd, cF], [f])
                    if is_k:
                        op(ACT, lambda h, i=i, ks=ks, f=f: h.activation(out=ks.ap[:, i * 512:(i + 1) * 512], in_=f.ap[:], func=AF.Copy), [f], [ks])
                        blk0 = p * 4 + i * 2
                        op(DVE, lambda h, blk0=blk0, f=f: h.tensor_reduce(out=kmean.ap[:, hd, blk0:blk0 + 2],
                                                                           in_=f.ap[:].rearrange("p (b k) -> p b k", b=2), axis=AX.X, op=ALU.add),
                           [f], [kmean])
                    else:
                        qc0 = 0 if p == 0 else 2 + i * 512
                        scratch_w.append(dma(SQ, qT[hd * 128:(hd + 1) * 128, qc0:qc0 + n], f.ap[:, 0:n], f, False))
                if is_k:
                    scratch_w.append(dma(SQ, kT[hd * 128:(hd + 1) * 128, p * HALF:(p + 1) * HALF], ks.ap[:], ks, False))

            for p in range(2):
                t0 = p * HALF
                for cc in range(4):
                    dma(SQ, xs.ap[:], kcv(xT)[:, :, t0 + cc * 256:t0 + (cc + 1) * 256], xs, True)
                    rmsnorm(lambda kc: xs.ap[:, kc, :], 256, C_GA, xn1, cc * 256, sqb, rstd, [xs])
                for hp in range(16):
                    wb = wload([(w_qkv[:, D + hp * 256:D + (hp + 1) * 256], 0)])
                    for hh in range(2):
                        qk_head(wb, hh * 128, C_KG, [(0, 512), (512, 512)], True, 2 * hp + hh, p)
                    wb = wload([(w_qkv[:, hp * 256:(hp + 1) * 256], 0)])
                    for hh in range(2):
                        qk_head(wb, hh * 128, C_QG, [(1022, 2)] if p == 0 else [(0, 512), (512, 512)], False, 2 * hp + hh, p)
                for vb in range(16):
                    wb = wload([(w_qkv[:, 2 * D + vb * 256:2 * D + (vb + 1) * 256], 0)])
                    vs = vst[vb % 2]
                    for tt in range(8):
                        s = slot2()
                        bk = banks[2 * s]

                        def fn(h, tt=tt, s=s, wb=wb):
                            last = None
                            for kc in range(NKC):
                                last = h.matmul(pst[:, 2 * s, 0:256], lhsT=xn1.ap[:, kc, tt * 128:(tt + 1) * 128], rhs=wb.ap[:, kc, :],
                                                start=(kc == 0), stop=(kc == NKC - 1))
                            return last
                        op(PE, fn, [wb, xn1], [bk])
                        op(ACT, lambda h, tt=tt, s=s, vs=vs: h.activation(out=vs.ap[:, tt, :], in_=pst[:, 2 * s, 0:256], func=AF.Copy), [bk], [vs])
                    scratch_w.append(dma(SQ, vS[t0:t0 + HALF, vb * 256:(vb + 1) * 256].rearrange("(t p) c -> p t c", p=128), vs.ap[:], vs, False))

        for gi in range(2):
            g0 = G0[gi]
            barrier()
            with ExitStack() as gs:
                acc = mk(gs, "acc", [128, NKC, NT], F32, True)
                act = mk(gs, "act", [128, NKC, NT], BF16)
                dma(SQ, acc.ap[:], kcv(xT)[:, :, g0:g0 + NT], acc, True)

                def linear_acc(w_ap, xbuf):
                    for blk in range(16):
                        wb = wload([(w_ap[:, blk * 256:(blk + 1) * 256], 0)])
                        for ft in range(2):
                            dm = 2 * blk + ft
                            s = slot2()
                            bks = mm_acc(wb, ft * 128, xbuf, NKC, s, CH2)
                            for i, (c0, n) in enumerate(CH2):
                                op(DVE, lambda h, i=i, c0=c0, n=n, dm=dm, s=s: h.tensor_tensor(out=acc.ap[:, dm, c0:c0 + n], in0=acc.ap[:, dm, c0:c0 + n],
                                                                                                  in1=pst[:, 2 * s + i, 0:n], op=ALU.add), [bks[i], acc], [acc])

                with ExitStack() as ph:
                    kTt = [mk(ph, "kTt%d" % i, [128, SEQ], BF16, True) for i in range(2)]
                    vh = [mk(ph, "vh%d" % i, [128, 16, 128], BF16, True) for i in range(2)]
                    qf = [mk(ph, "qf%d" % i, [128, NT], F32, True) for i in range(2)]
                    qb = mk(ph, "qb", [128, NT], BF16)
                    Dh = mk(ph, "Dh", [128, DW], F32)
                    b16 = mk(ph, "b16", [128, 16], F32)
                    sbb = [mk(ph, "sb%d" % i, [128, CK], F32) for i in range(2)]
                    pT = [mk(ph, "pT%d" % i, [128, CK], BF16) for i in range(2)]
                    selbT = mk(ph, "selbT", [8, NT], BF16)
                    gp = mk(ph, "gp", [128, 8], F32)
                    m8 = mk(ph, "m8", [128, 8], F32)
                    thr = mk(ph, "thr", [128, 1], F32)
                    selb = mk(ph, "selb", [128, 8], F32)
                    rinv = mk(ph, "rinv", [128, CK], F32)
                    qtiles = [(0, 2, g0 // 256)] + [(2 + 128 * i, 128, (g0 + 2 + 128 * i) // 256) for i in range(4)]
                    it = [0]
                    for hd in range(NH):
                        sl = hd % 2
                        m_h = alibi_slope(hd)
                        nkeys = g0 + NT
                        nkt_all = (nkeys + 127) // 128
                        dma(SQ, kTt[sl].ap[:, 0:nkt_all * 128], kT[hd * 128:(hd + 1) * 128, 0:nkt_all * 128], kTt[sl], True, extra=scratch_w)
                        dma(SQ, vh[sl].ap[:, 0:nkt_all, :], vS[0:nkt_all * 128, hd * 128:(hd + 1) * 128].rearrange("(t p) d -> p t d", p=128),
                            vh[sl], True, extra=scratch_w)
                        dma(SQ, qf[sl].ap[:], qT[hd * 128:(hd + 1) * 128, gi * 512:gi * 512 + NT], qf[sl], True, extra=scratch_w)
                        op(ACT, lambda h, sl=sl: h.activation(out=qb.ap[:], in_=qf[sl].ap[:], func=AF.Copy), [qf[sl]], [qb])
                        op(DVE, lambda h, m_h=m_h: h.tensor_scalar(out=Dh.ap[:], in0=c[:, C_D0:C_D0 + DW], scalar1=-m_h, scalar2=None, op0=ALU.mult),
                           [cF], [Dh])
                        op(DVE, lambda h, m_h=m_h: h.tensor_scalar(out=b16.ap[:], in0=c[:, C_IOTA:C_IOTA + 16], scalar1=-128.0 * m_h, scalar2=negB.ap[:, 0:1],
                                                                   op0=ALU.mult, op1=ALU.add), [cF, negB], [b16])
                        b7 = banks[7]
                        for (c0, nq, blk) in qtiles:
                            bi = BLKS.index(blk)
                            op(PE, lambda h, c0=c0, nq=nq, sl=sl, hd=hd: h.matmul(pst[0:nq, 7, 0:8], lhsT=qf[sl].ap[:, c0:c0 + nq], rhs=kmean.ap[:, hd, :],
                                                                                   start=True, stop=True), [qf[sl], kmean], [b7])
                            op(DVE, lambda h, nq=nq, bi=bi: h.tensor_tensor(out=gp.ap[0:nq, :], in0=pst[0:nq, 7, 0:8], in1=c[0:nq, C_PEN + 8 * bi:C_PEN + 8 * bi + 8],
                                                                            op=ALU.add), [b7, cF], [gp])
                            op(DVE, lambda h, nq=nq: h.max(out=m8.ap[0:nq, :], in_=gp.ap[0:nq, :]), [gp], [m8])
                            op(DVE, lambda h, nq=nq: h.tensor_scalar(out=thr.ap[0:nq, :], in0=m8.ap[0:nq, 2:3], scalar1=-5.0e8, scalar2=None, op0=ALU.max),
                               [m8], [thr])
                            op(DVE, lambda h, nq=nq: h.tensor_scalar(out=selb.ap[0:nq, :], in0=gp.ap[0:nq, :], scalar1=thr.ap[0:nq, 0:1], scalar2=None,
                                                                     op0=ALU.is_ge), [gp, thr], [selb])
                            op(DVE, lambda h, nq=nq, bi=bi: h.scalar_tensor_tensor(out=selb.ap[0:nq, :], in0=selb.ap[0:nq, :], scalar=-1.0,
                                                                                   in1=c[0:nq, C_BIGM + 8 * bi:C_BIGM + 8 * bi + 8], op0=ALU.add, op1=ALU.mult),
                               [selb, cF], [selb])
                            op(PE, lambda h, nq=nq: h.matmul(pst[0:8, 7, 16:16 + nq], lhsT=selb.ap[0:nq, :], rhs=c[0:nq, C_ID:C_ID + nq], start=True, stop=True),
                               [selb, cF], [b7])
                            op(ACT, lambda h, c0=c0, nq=nq: h.activation(out=selbT.ap[:, c0:c0 + nq], in_=pst[0:8, 7, 16:16 + nq], func=AF.Copy), [b7], [selbT])
                        for ck in range(2):
                            cc0 = ck * CK
                            q0 = g0 + cc0
                            nkt = (q0 + CK - 1) // 128 + 1
                            ob, sb_ = banks[3], banks[4]
                            tO = tS = None
                            for kt in range(nkt):
                                k0 = kt * 128
                                sbk = banks[kt % 3]
                                delta = q0 - k0
                                a = max(0, -(-(delta - 128) // 128))
                                dp = delta - 128 * a
                                coff = dp + 384
                                assert 0 <= coff and coff + CK <= DW, (coff, delta)
                                ii = it[0] % 2
                                it[0] += 1

                                def fs(h, kt=kt, k0=k0, sl=sl, cc0=cc0):
                                    h.matmul(pst[:, kt % 3, 0:CK], lhsT=kTt[sl].ap[:, k0:k0 + 128], rhs=qb.ap[:, cc0:cc0 + CK], start=True, stop=False)
                                    return h.matmul(pst[:, kt % 3, 0:CK], lhsT=E_bf(kt // 2), rhs=selbT.ap[:, cc0:cc0 + CK], start=False, stop=True)
                                op(PE, fs, [kTt[sl], qb, selbT, cB], [sbk])
                                op(DVE, lambda h, kt=kt, coff=coff, ii=ii: h.scalar_tensor_tensor(out=sbb[ii].ap[:], in0=pst[:, kt % 3, 0:CK], scalar=SCALE,
                                                                                                   in1=Dh.ap[:, coff:coff + CK], op0=ALU.mult, op1=ALU.add),
                                   [sbk, Dh], [sbb[ii]])
                                op(ACT, lambda h, ii=ii, a=a: h.activation(out=pT[ii].ap[:], in_=sbb[ii].ap[:], func=AF.Exp, bias=b16.ap[:, a:a + 1], scale=1.0),
                                   [sbb[ii], b16], [pT[ii]])
                                tO = op(PE, lambda h, kt=kt, sl=sl, ii=ii, nkt=nkt: h.matmul(pst[:, 3, 0:CK], lhsT=vh[sl].ap[:, kt, :], rhs=pT[ii].ap[:],
                                                                                             start=(kt == 0), stop=(kt == nkt - 1)),
                                        [vh[sl], pT[ii]], [ob] if kt == 0 else [])
                                tS = op(PE, lambda h, kt=kt, ii=ii, nkt=nkt: h.matmul(pst[:, 4, 0:CK], lhsT=ones_bf, rhs=pT[ii].ap[:],
                                                                                      start=(kt == 0), stop=(kt == nkt - 1)),
                                        [pT[ii], cB], [sb_] if kt == 0 else [])
                            ob.w, sb_.w = tO, tS
                            op(DVE, lambda h: h.reciprocal(out=rinv.ap[:], in_=pst[:, 4, 0:CK]), [sb_], [rinv])
                            op(DVE, lambda h, hd=hd, cc0=cc0: h.tensor_tensor(out=act.ap[:, hd, cc0:cc0 + CK], in0=pst[:, 3, 0:CK], in1=rinv.ap[:], op=ALU.mult),
                               [ob, rinv], [act])

                barrier()
                linear_acc(w_ao, act)

                def store_out(skip_wait=False):
                    t = dma(SQ, kcv(oT)[:, :, gi * 512:(gi + 1) * 512], acc.ap[:, :, 2:NT], acc, False)
                    state["final"].append(t)

                if PHASES <= 3:
                    store_out()
                    continue

                with ExitStack() as ph:
                    xn = mk(ph, "xn", [128, NKC, NT], BF16)
                    sq = [mk(ph, "sq%d" % i, [128, CK], F32) for i in range(2)]
                    rstd_g = mk(ph, "rstd", [128, CK], F32)
                    sg = [mk(ph, "sg%d" % i, [128, CK], F32) for i in range(2)]
                    cwbc = mk(ph, "cwbc", [128, NT], F32)
                    combT = mk(ph, "combT", [8, NT], F32)
                    ub = cwbc
                    yb = mk(ph, "yb", [128, NT], F32)
                    ctb = sg
                    lg = mk(ph, "lg", [128, 8], F32)
                    ex = mk(ph, "ex", [128, 8], F32)
                    m8r = mk(ph, "m8b", [128, 8], F32)
                    nv1 = mk(ph, "nv1", [128, 1], F32)
                    den = mk(ph, "den", [128, 1], F32)
                    sgi = [0]

                    def norm(gcol):
                        for (c0, n) in CH2:
                            rmsnorm(lambda kc, c0=c0, n=n: acc.ap[:, kc, c0:c0 + n], n, gcol, xn, c0, sq, rstd_g, [acc])

                    def mlp_slice(w_gu_ap, gcol0, ucol0, nft, w_dn_ap, cw):
                        for j in range(nft):
                            wb = wload([(w_gu_ap[:, gcol0 + j * 128:gcol0 + (j + 1) * 128], 0), (w_gu_ap[:, ucol0 + j * 128:ucol0 + (j + 1) * 128], 128)])
                            sgs = slot2()
                            gb = mm_acc(wb, 0, xn, NKC, sgs, CH2)
                            sus = slot2()
                            ubk = mm_acc(wb, 128, xn, NKC, sus, CH2)
                            for i, (c0, n) in enumerate(CH2):
                                g = sg[sgi[0] % 2]
                                sgi[0] += 1
                                op(ACT, lambda h, i=i, g=g, sgs=sgs: h.activation(out=g.ap[:], in_=pst[:, 2 * sgs + i, 0:CK], func=AF.Silu), [gb[i]], [g])
                                if cw is not None:
                                    op(DVE, lambda h, g=g, c0=c0: h.tensor_tensor(out=g.ap[:], in0=g.ap[:], in1=cw.ap[:, c0:c0 + CK], op=ALU.mult), [g, cw], [g])
                                op(DVE, lambda h, i=i, g=g, j=j, c0=c0, sus=sus: h.tensor_tensor(out=act.ap[:, j, c0:c0 + CK], in0=g.ap[:], in1=pst[:, 2 * sus + i, 0:CK],
                                                                                                   op=ALU.mult), [g, ubk[i]], [act])
                        for blk in range(16):
                            wb = wload([(w_dn_ap[:, blk * 256:(blk + 1) * 256], 0)], nkc=nft)
                            for ft in range(2):
                                dm = 2 * blk + ft
                                s = slot2()
                                bks = mm_acc(wb, ft * 128, act, nft, s, CH2)
                                for i, (c0, n) in enumerate(CH2):
                                    op(DVE, lambda h, i=i, c0=c0, n=n, dm=dm, s=s: h.tensor_tensor(out=acc.ap[:, dm, c0:c0 + n], in0=acc.ap[:, dm, c0:c0 + n],
                                                                                                      in1=pst[:, 2 * s + i, 0:n], op=ALU.add), [bks[i], acc], [acc])

                    norm(C_GF)
                    for sidx in range(4):
                        mlp_slice(w_gu, sidx * 3584, DFF + sidx * 3584, 28, w_dn[sidx * 3584:(sidx + 1) * 3584, :], None)
                    if PHASES <= 4:
                        store_out()
                        continue

                    norm(C_GC)
                    for j in range(NKC):
                        wb = wload([(w_ci[:, D + j * 128:D + (j + 1) * 128], 0), (w_ci[:, 2 * D + j * 128:2 * D + (j + 1) * 128], 128)])
                        scs = slot2()
                        cb = mm_acc(wb, 0, xn, NKC, scs, CH2)
                        shs = slot2()
                        hb = mm_acc(wb, 128, xn, NKC, shs, CH2)
                        wb2 = wload([(w_ci[:, j * 128:(j + 1) * 128], 0)])
                        sbs = slot2()
                        bb = mm_acc(wb2, 0, xn, NKC, sbs, CH2)
                        for i, (c0, n) in enumerate(CH2):
                            ct = ctb[i]
                            op(ACT, lambda h, i=i, ct=ct, scs=scs: h.activation(out=ct.ap[:], in_=pst[:, 2 * scs + i, 0:CK], func=AF.Copy), [cb[i]], [ct])
                            op(DVE, lambda h, i=i, ct=ct, c0=c0, shs=shs: h.tensor_tensor(out=ub.ap[:, c0:c0 + CK], in0=ct.ap[:], in1=pst[:, 2 * shs + i, 0:CK], op=ALU.mult),
                               [ct, hb[i]], [ub])
                        cwc = C_CW
                        op(DVE, lambda h, j=j: h.tensor_scalar(out=yb.ap[:], in0=ub.ap[:], scalar1=c[:, cwc + 64 + j:cwc + 65 + j], scalar2=None, op0=ALU.mult), [ub, cF], [yb])
                        op(DVE, lambda h, j=j: h.scalar_tensor_tensor(out=yb.ap[:, 1:NT], in0=ub.ap[:, 0:NT - 1], scalar=c[:, cwc + 32 + j:cwc + 33 + j], in1=yb.ap[:, 1:NT],
                                                                       op0=ALU.mult, op1=ALU.add), [ub, cF, yb], [yb])
                        op(DVE, lambda h, j=j: h.scalar_tensor_tensor(out=yb.ap[:, 2:NT], in0=ub.ap[:, 0:NT - 2], scalar=c[:, cwc + j:cwc + 1 + j], in1=yb.ap[:, 2:NT],
                                                                       op0=ALU.mult, op1=ALU.add), [ub, cF, yb], [yb])
                        for i, (c0, n) in enumerate(CH2):
                            op(DVE, lambda h, i=i, j=j, c0=c0, sbs=sbs: h.tensor_tensor(out=act.ap[:, j, c0:c0 + CK], in0=yb.ap[:, c0:c0 + CK], in1=pst[:, 2 * sbs + i, 0:CK],
                                                                                        op=ALU.mult), [yb, bb[i]], [act])
                    linear_acc(w_co, act)
                    if PHASES <= 6:
                        store_out()
                        continue

                    norm(C_GM)
                    op(DVE, lambda h: h.memset(combT.ap[:, 0:2], 0.0), [], [combT])
                    for i4 in range(4):
                        c0 = 2 + 128 * i4
                        s = slot2()
                        bk = banks[2 * s]

                        def fr(h, c0=c0, s=s):
                            last = None
                            for kc in range(NKC):
                                last = h.matmul(pst[:, 2 * s, 0:8], lhsT=xn.ap[:, kc, c0:c0 + 128], rhs=wr_bf.ap[:, kc * NE:(kc + 1) * NE],
                                                start=(kc == 0), stop=(kc == NKC - 1))
                            return last
                        op(PE, fr, [xn, wr_bf], [bk])
                        op(DVE, lambda h, s=s: h.tensor_copy(out=lg.ap[:], in_=pst[:, 2 * s, 0:8]), [bk], [lg])
                        op(DVE, lambda h: h.max(out=m8r.ap[:], in_=lg.ap[:]), [lg], [m8r])
                        op(DVE, lambda h: h.tensor_scalar(out=nv1.ap[:], in0=m8r.ap[:, 0:1], scalar1=-1.0, scalar2=None, op0=ALU.mult), [m8r], [nv1])
                        op(ACT, lambda h: h.activation(out=ex.ap[:], in_=lg.ap[:], func=AF.Exp, bias=nv1.ap[:, 0:1], scale=1.0), [lg, nv1], [ex])
                        op(DVE, lambda h: h.tensor_scalar(out=lg.ap[:], in0=lg.ap[:], scalar1=m8r.ap[:, 1:2], scalar2=None, op0=ALU.is_ge), [lg, m8r, ex], [lg])
                        op(DVE, lambda h: h.tensor_tensor(out=ex.ap[:], in0=ex.ap[:], in1=lg.ap[:], op=ALU.mult), [lg, ex], [ex])
                        op(DVE, lambda h: h.tensor_reduce(out=den.ap[:], in_=ex.ap[:], axis=AX.X, op=ALU.add), [ex], [den])
                        op(DVE, lambda h: h.reciprocal(out=den.ap[:], in_=den.ap[:]), [den], [den])
                        op(DVE, lambda h: h.tensor_scalar(out=ex.ap[:], in0=ex.ap[:], scalar1=den.ap[:, 0:1], scalar2=None, op0=ALU.mult), [ex, den], [ex])
                        op(PE, lambda h, s=s: h.matmul(pst[0:8, 2 * s + 1, 0:128], lhsT=ex.ap[:], rhs=ident, start=True, stop=True), [ex, cF], [banks[2 * s + 1]])
                        op(ACT, lambda h, s=s, c0=c0: h.activation(out=combT.ap[:, c0:c0 + 128], in_=pst[0:8, 2 * s + 1, 0:128], func=AF.Copy),
                           [banks[2 * s + 1]], [combT])
                    for e in range(NE):
                        s = slot2()
                        bks = [banks[2 * s], banks[2 * s + 1]]
                        for i, (c0, n) in enumerate(CH2):
                            op(PE, lambda h, i=i, c0=c0, e=e, s=s: h.matmul(pst[:, 2 * s + i, 0:CK], lhsT=E_f(e), rhs=combT.ap[:, c0:c0 + CK], start=True, stop=True),
                               [combT, cF], [bks[i]])
                            op(ACT, lambda h, i=i, c0=c0, s=s: h.activation(out=cwbc.ap[:, c0:c0 + CK], in_=pst[:, 2 * s + i, 0:CK], func=AF.Copy), [bks[i]], [cwbc])
                        for hf in range(2):
                            mlp_slice(w_egu[e * D:(e + 1) * D, :], hf * 2816, DFE + hf * 2816, 22,
                                      w_edn[e * DFE + hf * 2816:e * DFE + (hf + 1) * 2816, :], cwbc)
                    store_out()

        SQ.wait(state["final"])
        engs = {"tensor": PE, "scalar": ACT, "vector": DVE, "gpsimd": GQ, "sync": SQ}
        with nc.Block() as block:
            for name, e in engs.items():
                def body(h, e=e):
                    for f in e.prog:
                        f(h)
                getattr(block, name)(body)
    return nc


def make_consts(core, inputs):
    cf = np.zeros((128, NF), np.float32)
    cf[:, C_ONESD:C_ONESD + 128] = 1.0 / D
    cf[:, C_ONESH:C_ONESH + 128] = 1.0 / 128.0
    cf[:, C_ID:C_ID + 128] = np.eye(128, dtype=np.float32)
    p = np.arange(128)[:, None]
    cc = np.arange(1024)[None, :]
    dist = (cc - p - 384).astype(np.float32)
    cf[:, C_D0:C_D0 + 1024] = np.where(dist >= 0, dist, np.float32(1e34))
    cf[:, C_IOTA:C_IOTA + 16] = np.arange(16, dtype=np.float32)[None, :]
    pen_core = -1.0e9 if core % 2 == 0 else 0.0
    for bi, blk in enumerate(BLKS):
        n = np.arange(8)
        pen = np.where(n < 4, pen_core, 0.0) + np.where(n >= blk, -1.0e9, 0.0)
        cf[:, C_PEN + 8 * bi:C_PEN + 8 * bi + 8] = pen[None, :]
        cf[:, C_BIGM + 8 * bi:C_BIGM + 8 * bi + 8] = np.where(n == blk, 0.0, BIG2)[None, :]
    for n in range(8):
        cf[n, C_EF + n * 128:C_EF + (n + 1) * 128] = 1.0
    for col, key in ((C_GA, "attn_norm"), (C_GF, "ffn_norm"), (C_GC, "conv_norm"), (C_GM, "moe_norm")):
        cf[:, col:col + 32] = inputs[key][0].reshape(32, 128).T
    cf[:, C_QG] = inputs["q_norm"][0]
    cf[:, C_KG] = inputs["k_norm"][0]
    cw = inputs["conv_w"][0]
    for k in range(3):
        cf[:, C_CW + 32 * k:C_CW + 32 * k + 32] = cw[k].reshape(32, 128).T
    cf[:, C_WR:C_WR + 256] = inputs["w_router"][0].reshape(32, 128, 8).transpose(1, 0, 2).reshape(128, 256)
    cf[:, C_ONEROW:C_ONEROW + 128] = 1.0
    cf[:, C_EPS] = EPS
    cb = np.zeros((128, 128 + 1024), np.float32)
    cb[:, 0:128] = 1.0
    for n in range(8):
        cb[n, 128 + n * 128:128 + (n + 1) * 128] = 1.0
    return cf, cb


_NC_CACHE = {}


def kernel(**inputs):
    inputs = {k: np.asarray(v) for k, v in inputs.items()}
    x = inputs["x"]
    if "nc" not in _NC_CACHE:
        _NC_CACHE["nc"] = build_nc()
    nc = _NC_CACHE["nc"]
    shared = {
        "w_qkv": inputs["w_qkv"][0], "w_ao": inputs["w_attn_out"][0], "w_gu": inputs["w_ffn_gate_up"][0],
        "w_dn": inputs["w_ffn_down"][0], "w_ci": inputs["w_conv_in"][0], "w_co": inputs["w_conv_out"][0],
        "w_egu": inputs["w_expert_gate_up"][0].reshape(NE * D, 2 * DFE), "w_edn": inputs["w_expert_down"][0].reshape(NE * DFE, D),
    }
    in_maps = []
    for core in CORES:
        b, hf = core // 2, core % 2
        xw = np.zeros((D, SEQ), np.float32)
        if hf == 1:
            xw[:, :] = x[b].T
        else:
            xw[:, HALF:] = x[b, :HALF].T
        cf, cb = make_consts(core, inputs)
        m = dict(shared)
        m.update({"xT": xw, "cF": cf, "cB": cb})
        in_maps.append(m)
    res = run_bass_kernel_spmd(nc, in_maps, core_ids=list(range(len(CORES))))
    out = np.zeros_like(x)
    for i, core in enumerate(CORES):
        b, hf = core // 2, core % 2
        out[b, hf * HALF:(hf + 1) * HALF, :] = res.results[i]["oT"].T
    return out
```
